# Optimizing a Trainium2 kernel written in Bass

```python
import jax, jax.numpy as jnp
from jax import lax
import numpy as np

D_MODEL = 1024
BATCH = 8
SEQ = 4096
DEPTH = 4

MLA_HEADS = 8
MLA_NOPE_DIM = 64
MLA_ROPE_DIM = 32
MLA_V_DIM = 64
MLA_Q_RANK = 256
MLA_KV_RANK = 128
ROPE_THETA = 10000.0
FOX_HEADS = 4
FOX_HEAD_DIM = 64
CONV_CHANNELS = 256
CONV_GROUPS = 4
CONV_WIDTH = 31
MLA_WIDTH = MLA_HEADS * MLA_V_DIM
FOX_WIDTH = FOX_HEADS * FOX_HEAD_DIM
MIX_WIDTH = MLA_WIDTH + FOX_WIDTH + CONV_CHANNELS
IN_SIZES = (MLA_Q_RANK, MLA_KV_RANK, MLA_ROPE_DIM, FOX_WIDTH, FOX_WIDTH, FOX_WIDTH, FOX_HEADS, 2 * CONV_CHANNELS)
IN_COLS = sum(IN_SIZES)
D_FF_DENSE = 2816
N_EXPERTS = 8
TOP_K = 2
D_FF_EXPERT = 1408
Q_BLOCK = 128
ALPHA = (2.0 * DEPTH) ** 0.25
BETA = (8.0 * DEPTH) ** -0.25
NORM_EPS = 1e-5

kernel_name = 'hybrid_mla_fox_conformer_moe_deepnorm'


def layer_norm(x, g, b):
    xf = x.astype(jnp.float32)
    mu = jnp.mean(xf, axis=-1, keepdims=True)
    var = jnp.mean(jnp.square(xf - mu), axis=-1, keepdims=True)
    return ((xf - mu) * lax.rsqrt(var + NORM_EPS) * g.astype(jnp.float32) + b.astype(jnp.float32)).astype(x.dtype)


def rms_norm(x, g):
    xf = x.astype(jnp.float32)
    ms = jnp.mean(jnp.square(xf), axis=-1, keepdims=True)
    return (xf * lax.rsqrt(ms + NORM_EPS) * g.astype(jnp.float32)).astype(x.dtype)


def group_norm_per_position(x, g, b):
    bsz, s, c = x.shape
    xf = x.astype(jnp.float32).reshape(bsz, s, CONV_GROUPS, c // CONV_GROUPS)
    mu = jnp.mean(xf, axis=-1, keepdims=True)
    var = jnp.mean(jnp.square(xf - mu), axis=-1, keepdims=True)
    xn = ((xf - mu) * lax.rsqrt(var + NORM_EPS)).reshape(bsz, s, c)
    return (xn * g.astype(jnp.float32) + b.astype(jnp.float32)).astype(x.dtype)


def rope_tables(positions):
    inv_freq = ROPE_THETA ** (-jnp.arange(0, MLA_ROPE_DIM, 2, dtype=jnp.float32) / MLA_ROPE_DIM)
    ang = positions.astype(jnp.float32)[..., None] * inv_freq
    return jnp.cos(ang), jnp.sin(ang)


def apply_rope(x, cos, sin):
    x1, x2 = jnp.split(x.astype(jnp.float32), 2, axis=-1)
    return jnp.concatenate([x1 * cos - x2 * sin, x1 * sin + x2 * cos], axis=-1).astype(x.dtype)


def causal_block_attention(q, k, v, scale, log_decay_cum=None):
    bsz, s, h, _ = q.shape
    n_blocks = s // Q_BLOCK
    kf = k.astype(jnp.float32)
    vf = v.astype(jnp.float32)
    k_pos = jnp.arange(s)
    cum_t = None if log_decay_cum is None else jnp.transpose(log_decay_cum.astype(jnp.float32), (0, 2, 1))

    def one_block(i):
        start = i * Q_BLOCK
        qb = lax.dynamic_slice_in_dim(q, start, Q_BLOCK, axis=1).astype(jnp.float32)
        logits = jnp.einsum('bqhd,bkhd->bhqk', qb, kf) * scale
        if cum_t is not None:
            cq = lax.dynamic_slice_in_dim(cum_t, start, Q_BLOCK, axis=2)
            logits = logits + cq[..., None] - cum_t[:, :, None, :]
        q_pos = start + jnp.arange(Q_BLOCK)
        mask = k_pos[None, :] <= q_pos[:, None]
        logits = jnp.where(mask, logits, -jnp.inf)
        p = jax.nn.softmax(logits, axis=-1)
        return jnp.einsum('bhqk,bkhd->bqhd', p, vf)

    out = lax.map(one_block, jnp.arange(n_blocks))
    out = jnp.moveaxis(out, 0, 1).reshape(bsz, s, h, vf.shape[-1])
    return out.astype(v.dtype)


def hybrid_mixer(x, cos, sin, w_in, mla_q_norm_g, w_uq, mla_kv_norm_g, w_ukv, fox_forget_b,
                 conv_w, conv_b, conv_norm_g, conv_norm_b, mla_out_norm_g, fox_out_norm_g, w_out):
    bsz, s, _ = x.shape
    proj = jnp.einsum('bsd,dc->bsc', x, w_in)
    offsets = np.cumsum(IN_SIZES)[:-1].tolist()
    c_q, c_kv, k_rope, fq, fk, fv, f_logit, conv_in = jnp.split(proj, offsets, axis=-1)

    q = jnp.einsum('bsr,rc->bsc', rms_norm(c_q, mla_q_norm_g), w_uq)
    q = q.reshape(bsz, s, MLA_HEADS, MLA_NOPE_DIM + MLA_ROPE_DIM)
    q_nope, q_rope = q[..., :MLA_NOPE_DIM], q[..., MLA_NOPE_DIM:]
    kv = jnp.einsum('bsr,rc->bsc', rms_norm(c_kv, mla_kv_norm_g), w_ukv)
    kv = kv.reshape(bsz, s, MLA_HEADS, MLA_NOPE_DIM + MLA_V_DIM)
    k_nope, v_mla = kv[..., :MLA_NOPE_DIM], kv[..., MLA_NOPE_DIM:]
    q_rope = apply_rope(q_rope, cos[:, :, None, :], sin[:, :, None, :])
    k_rope = apply_rope(k_rope, cos, sin)
    q_mla = jnp.concatenate([q_nope, q_rope], axis=-1)
    k_mla = jnp.concatenate(
        [k_nope, jnp.broadcast_to(k_rope[:, :, None, :], (bsz, s, MLA_HEADS, MLA_ROPE_DIM))], axis=-1)
    mla = causal_block_attention(q_mla, k_mla, v_mla, (MLA_NOPE_DIM + MLA_ROPE_DIM) ** -0.5)
    mla = mla.reshape(bsz, s, MLA_WIDTH)

    log_f = jax.nn.log_sigmoid(f_logit.astype(jnp.float32) + fox_forget_b.astype(jnp.float32))
    cum = jnp.cumsum(log_f, axis=1)
    fox = causal_block_attention(
        fq.reshape(bsz, s, FOX_HEADS, FOX_HEAD_DIM),
        fk.reshape(bsz, s, FOX_HEADS, FOX_HEAD_DIM),
        fv.reshape(bsz, s, FOX_HEADS, FOX_HEAD_DIM),
        FOX_HEAD_DIM ** -0.5, cum)
    fox = fox.reshape(bsz, s, FOX_WIDTH)

    a, g = jnp.split(conv_in, 2, axis=-1)
    h = a * jax.nn.sigmoid(g)
    h = lax.conv_general_dilated(
        h, conv_w[:, None, :], window_strides=(1,), padding=[(CONV_WIDTH - 1, 0)],
        dimension_numbers=('NWC', 'WIO', 'NWC'), feature_group_count=CONV_CHANNELS) + conv_b
    h = jax.nn.silu(group_norm_per_position(h, conv_norm_g, conv_norm_b))

    mixed = jnp.concatenate([rms_norm(mla, mla_out_norm_g), rms_norm(fox, fox_out_norm_g), h], axis=-1)
    return jnp.einsum('bsc,cd->bsd', mixed, w_out)


def swiglu(x, w1, w3, w2):
    hid = jax.nn.silu(jnp.einsum('bsd,df->bsf', x, w1)) * jnp.einsum('bsd,df->bsf', x, w3)
    return jnp.einsum('bsf,fd->bsd', hid, w2)


def moe_swiglu(x, router_w, w1, w3, w2):
    logits = jnp.einsum('bsd,de->bse', x, router_w).astype(jnp.float32)
    top_val, top_idx = lax.top_k(logits, TOP_K)
    top_w = jax.nn.softmax(top_val, axis=-1)
    gates = jnp.sum(jax.nn.one_hot(top_idx, N_EXPERTS, dtype=jnp.float32) * top_w[..., None], axis=-2)
    y = jnp.zeros_like(x)
    for e in range(N_EXPERTS):
        y = y + gates[..., e:e + 1].astype(x.dtype) * swiglu(x, w1[e], w3[e], w2[e])
    return y


def setup_inputs(seed: int = 0) -> dict:
    key = jax.random.key(seed)
    keys = jax.random.split(key, 40)
    ks = [keys[i] for i in range(40)]
    f32 = jnp.float32
    n_dense = (DEPTH + 1) // 2
    n_moe = DEPTH // 2

    def nrm(shape, scale):
        return jax.random.normal(ks.pop(), shape, f32) * scale

    def gain(shape):
        return 1.0 + 0.02 * jax.random.normal(ks.pop(), shape, f32)

    x = jax.random.normal(ks.pop(), (BATCH, SEQ, D_MODEL), f32)
    positions = jnp.tile(jnp.arange(SEQ, dtype=jnp.int32)[None, :], (BATCH, 1))
    return {
        'x': x,
        'positions': positions,
        'w_in': nrm((DEPTH, D_MODEL, IN_COLS), D_MODEL ** -0.5),
        'mla_q_norm_g': gain((DEPTH, MLA_Q_RANK)),
        'w_uq': nrm((DEPTH, MLA_Q_RANK, MLA_HEADS * (MLA_NOPE_DIM + MLA_ROPE_DIM)), MLA_Q_RANK ** -0.5),
        'mla_kv_norm_g': gain((DEPTH, MLA_KV_RANK)),
        'w_ukv': nrm((DEPTH, MLA_KV_RANK, MLA_HEADS * (MLA_NOPE_DIM + MLA_V_DIM)), MLA_KV_RANK ** -0.5),
        'fox_forget_b': jax.random.uniform(ks.pop(), (DEPTH, FOX_HEADS), f32, minval=1.0, maxval=5.0),
        'conv_w': nrm((DEPTH, CONV_WIDTH, CONV_CHANNELS), CONV_WIDTH ** -0.5),
        'conv_b': nrm((DEPTH, CONV_CHANNELS), 0.02),
        'conv_norm_g': gain((DEPTH, CONV_CHANNELS)),
        'conv_norm_b': nrm((DEPTH, CONV_CHANNELS), 0.02),
        'mla_out_norm_g': gain((DEPTH, MLA_WIDTH)),
        'fox_out_norm_g': gain((DEPTH, FOX_WIDTH)),
        'w_out': nrm((DEPTH, MIX_WIDTH, D_MODEL), BETA * MIX_WIDTH ** -0.5),
        'ln1_g': gain((DEPTH, D_MODEL)),
        'ln1_b': nrm((DEPTH, D_MODEL), 0.02),
        'dense_w1': nrm((n_dense, D_MODEL, D_FF_DENSE), D_MODEL ** -0.5),
        'dense_w3': nrm((n_dense, D_MODEL, D_FF_DENSE), D_MODEL ** -0.5),
        'dense_w2': nrm((n_dense, D_FF_DENSE, D_MODEL), BETA * D_FF_DENSE ** -0.5),
        'router_w': nrm((n_moe, D_MODEL, N_EXPERTS), D_MODEL ** -0.5),
        'expert_w1': nrm((n_moe, N_EXPERTS, D_MODEL, D_FF_EXPERT), D_MODEL ** -0.5),
        'expert_w3': nrm((n_moe, N_EXPERTS, D_MODEL, D_FF_EXPERT), D_MODEL ** -0.5),
        'expert_w2': nrm((n_moe, N_EXPERTS, D_FF_EXPERT, D_MODEL), BETA * D_FF_EXPERT ** -0.5),
        'ln2_g': gain((DEPTH, D_MODEL)),
        'ln2_b': nrm((DEPTH, D_MODEL), 0.02),
    }


def reference(x, positions, w_in, mla_q_norm_g, w_uq, mla_kv_norm_g, w_ukv, fox_forget_b,
              conv_w, conv_b, conv_norm_g, conv_norm_b, mla_out_norm_g, fox_out_norm_g, w_out,
              ln1_g, ln1_b, dense_w1, dense_w3, dense_w2, router_w, expert_w1, expert_w3,
              expert_w2, ln2_g, ln2_b):
    cos, sin = rope_tables(positions)
    for layer in range(DEPTH):
        mix = hybrid_mixer(x, cos, sin, w_in[layer], mla_q_norm_g[layer], w_uq[layer],
                           mla_kv_norm_g[layer], w_ukv[layer], fox_forget_b[layer],
                           conv_w[layer], conv_b[layer], conv_norm_g[layer], conv_norm_b[layer],
                           mla_out_norm_g[layer], fox_out_norm_g[layer], w_out[layer])
        x = layer_norm(ALPHA * x + mix, ln1_g[layer], ln1_b[layer])
        j = layer // 2
        if layer % 2 == 0:
            ff = swiglu(x, dense_w1[j], dense_w3[j], dense_w2[j])
        else:
            ff = moe_swiglu(x, router_w[j], expert_w1[j], expert_w3[j], expert_w2[j])
        x = layer_norm(ALPHA * x + ff, ln2_g[layer], ln2_b[layer])
    return x
```

```python
import contextlib
import math
import numpy as np
import concourse.bass as bass
import concourse.mybir as mybir
from concourse.bass_utils import run_bass_kernel_spmd

F32 = mybir.dt.float32
BF16 = mybir.dt.bfloat16
I32 = mybir.dt.int32
AF = mybir.ActivationFunctionType
ALU = mybir.AluOpType

D = 1024
SEQ = 4096
TC = 512
NCH = SEQ // TC
DEPTH = 4
ALPHA = (2.0 * DEPTH) ** 0.25
EPS = 1e-5
NP = 110
WIN = 1984
PI = math.pi


class Buf:
    __slots__ = ("w", "r")

    def __init__(self):
        self.w = None
        self.r = {}


class Op:
    __slots__ = ("eng", "fn", "deps", "signal", "val", "sem", "is_dma", "idx")


class Sched:
    ENGS = ("pe", "act", "dve", "pool", "sp")

    def __init__(self, nc, n_dma_sems=8):
        self.nc = nc
        self.ops = {e: [] for e in self.ENGS}
        self.n_dma_sems = n_dma_sems
        self.dma_rr = {e: 0 for e in self.ENGS}
        self.dma_last = {}
        self.nops = 0
        self.ALL = Buf()
        self._cap = None
        self.tagged = []

    def capture(self, f):
        assert self._cap is None
        self._cap = []
        f()
        lst = self._cap
        self._cap = None
        return lst

    def replay(self, lst, n):
        for _ in range(min(n, len(lst))):
            a = lst.pop(0)
            self.op(a[0], a[1], a[2], a[3], dma=a[4], tag=a[5])

    def op(self, eng, fn, reads=(), writes=(), dma=False, barrier=False, free=False, tag=None):
        if self._cap is not None:
            assert not barrier
            self._cap.append((eng, fn, list(reads), list(writes), dma, tag))
            return None
        o = Op()
        o.eng = eng; o.fn = fn; o.signal = False; o.val = None; o.sem = None
        o.is_dma = dma; o.idx = self.nops; self.nops += 1
        deps = []
        reads = list(reads); writes = list(writes)
        if barrier:
            writes.append(self.ALL)
        elif not free:
            reads.append(self.ALL)
        for b in reads:
            if b.w is not None:
                deps.append(b.w)
        for b in writes:
            if b.w is not None:
                deps.append(b.w)
            deps.extend(b.r.values())
        if dma:
            i = self.dma_rr[eng]
            self.dma_rr[eng] = (i + 1) % self.n_dma_sems
            o.sem = (eng, i)
            prev = self.dma_last.get((eng, i))
            if prev is not None:
                deps.append(prev)
            self.dma_last[(eng, i)] = o
            o.signal = True
        seen = set(); dd = []
        for d in deps:
            if d is o or id(d) in seen:
                continue
            seen.add(id(d))
            if d.eng == "pe" and eng == "pe" and not d.is_dma and not dma:
                continue
            dd.append(d)
            d.signal = True
        o.deps = dd
        for b in writes:
            b.w = o; b.r = {}
        for b in reads:
            key = o.sem if dma else eng
            b.r[key] = o
        self.ops[eng].append(o)
        if tag is not None:
            self.tagged.append(o)
        return o

    def barrier(self):
        return self.op("sp", lambda e: e.nop(), barrier=True)

    def emit(self, final_waits=()):
        nc = self.nc
        with contextlib.ExitStack() as st:
            sems = {}
            for e in self.ENGS:
                sems[e] = st.enter_context(nc.semaphore("s_" + e))
                for i in range(self.n_dma_sems):
                    if (e, i) in self.dma_last:
                        sems[(e, i)] = st.enter_context(nc.semaphore("d_%s%d" % (e, i)))
            cnt = {}
            for e in self.ENGS:
                for o in self.ops[e]:
                    if not o.signal:
                        continue
                    key = o.sem if o.is_dma else e
                    inc = 16 if o.is_dma else 1
                    cnt[key] = cnt.get(key, 0) + inc
                    o.val = cnt[key]
                    o.sem = key
            block = st.enter_context(nc.Block())
            engmap = {"pe": "tensor", "act": "scalar", "dve": "vector", "pool": "gpsimd", "sp": "sync"}

            def make(e):
                def body(eng):
                    waited = {}
                    for o in self.ops[e]:
                        for d in o.deps:
                            if waited.get(d.sem, 0) >= d.val:
                                continue
                            waited[d.sem] = d.val
                            eng.wait_ge(sems[d.sem], d.val)
                        ins = o.fn(eng)
                        if o.signal:
                            ins.then_inc(sems[o.sem], 16 if o.is_dma else 1)
                    if e == "sp":
                        for d in final_waits:
                            if waited.get(d.sem, 0) >= d.val:
                                continue
                            waited[d.sem] = d.val
                            eng.wait_ge(sems[d.sem], d.val)
                return body

            for e in self.ENGS:
                if self.ops[e] or e == "sp":
                    getattr(block, engmap[e])(make(e))


def MM(out, lhsT, rhs, st, sp):
    return lambda e: e.matmul(out, lhsT, rhs, start=st, stop=sp, skip_group_check=True)


def ACT(out, in_, func, bias=None, scale=None):
    kw = {}
    if bias is not None:
        kw["bias"] = bias
    if scale is not None:
        kw["scale"] = scale
    return lambda e: e.activation(out=out, in_=in_, func=func, **kw)


def STT(out, in0, scalar, in1, op0, op1):
    return lambda e: e.scalar_tensor_tensor(out=out, in0=in0, scalar=scalar, in1=in1, op0=op0, op1=op1)


def TT(out, in0, in1, op):
    return lambda e: e.tensor_tensor(out=out, in0=in0, in1=in1, op=op)


def TS(out, in0, s1, s2, op0, op1=None):
    if op1 is None:
        return lambda e: e.tensor_scalar(out=out, in0=in0, scalar1=s1, scalar2=None, op0=op0)
    return lambda e: e.tensor_scalar(out=out, in0=in0, scalar1=s1, scalar2=s2, op0=op0, op1=op1)


def CP(out, in_):
    return lambda e: e.tensor_copy(out=out, in_=in_)


def RECIP(out, in_):
    return lambda e: e.reciprocal(out=out, in_=in_)


def MEMSET(ap, v):
    return lambda e: e.memset(ap, v)


def DMA(out, in_):
    return lambda e: e.dma_start(out=out, in_=in_)


PC_QG, PC_KVG, PC_FB, PC_CW, PC_CB, PC_CNG, PC_CNB, PC_MOG, PC_FOG = 0, 2, 3, 4, 66, 68, 70, 72, 76
PC_L1G, PC_L1B, PC_L2G, PC_L2B = 78, 86, 94, 102

CB_ID, CB_NM, CB_ONE, CB_BD, CB_SQ, CB_SK = 0, 128, 256, 384, 512, 512 + 4 * 68
CB_N = 512 + 8 * 68
CF_ONE, CF_ID, CF_IF, CF_NS, CF_SE = 0, 128, 256, 257, 258
CF_N = 258 + 1024


class Prog:
    def __init__(self, n_layers=DEPTH, dbg=None):
        self.n_layers = n_layers
        self.dbg = dbg
        nc = self.nc = bass.Bass("TRN2", target_bir_lowering=False)
        self.S = Sched(nc)
        dt = nc.dram_tensor
        self.xT = dt("xT", [8, 128, SEQ], F32, kind="ExternalInput").ap()
        self.posrep = dt("posrep", [128, SEQ], I32, kind="ExternalInput").ap()
        self.w_in = dt("w_inL", [DEPTH, D, WIN], F32, kind="ExternalInput").ap()
        self.w_uq = dt("w_uqL", [DEPTH, 256, 1536], F32, kind="ExternalInput").ap()
        self.w_ukv = dt("w_ukv", [DEPTH, 128, 1024], F32, kind="ExternalInput").ap()
        self.w_out = dt("w_out", [DEPTH, D, D], F32, kind="ExternalInput").ap()
        self.dw1 = dt("dense_w1", [2, D, 2816], F32, kind="ExternalInput").ap()
        self.dw3 = dt("dense_w3", [2, D, 2816], F32, kind="ExternalInput").ap()
        self.dw2 = dt("dense_w2", [2, 2816, D], F32, kind="ExternalInput").ap()
        self.rw = dt("router_w", [2, D, 8], F32, kind="ExternalInput").ap()
        self.ew1 = dt("expert_w1", [2, 8, D, 1408], F32, kind="ExternalInput").ap()
        self.ew3 = dt("expert_w3", [2, 8, D, 1408], F32, kind="ExternalInput").ap()
        self.ew2 = dt("expert_w2", [2, 8, 1408, D], F32, kind="ExternalInput").ap()
        self.pcols_d = dt("pcols", [128, DEPTH * NP], F32, kind="ExternalInput").ap()
        self.cstb_d = dt("cstb", [128, CB_N], F32, kind="ExternalInput").ap()
        self.cstf_d = dt("cstf", [128, CF_N], F32, kind="ExternalInput").ap()
        self.outT = dt("outT", [8, 128, SEQ], F32, kind="ExternalOutput").ap()
        self.xres = dt("xres", [8, 128, SEQ], F32).ap()
        self.sWin = dt("sWin", [DEPTH, 128, 8, WIN], BF16).ap()
        self.sWuq = dt("sWuq", [DEPTH, 128, 2, 1536], BF16).ap()
        self.sWukv = dt("sWukv", [DEPTH, 128, 1024], BF16).ap()
        self.sWout = dt("sWout", [DEPTH, 128, 8, D], BF16).ap()
        self.sD13 = dt("sD13", [2, 2, 2, 11, 128, 8, 128], BF16).ap()
        self.sD2 = dt("sD2", [2, 2, 8, 128, 11, 128], BF16).ap()
        self.sE13 = dt("sE13", [2, 8, 2, 11, 128, 8, 128], BF16).ap()
        self.sE2 = dt("sE2", [2, 8, 8, 128, 11, 128], BF16).ap()
        self.wb = {}
        self.b_xres = [[Buf() for _ in range(NCH)] for _ in range(8)]
        self.b_out = Buf()
        self.out_ops = []
        self.dbg_outs = {}

        ARENA_BYTES = 212000
        self.arena = nc.alloc_sbuf_tensor("arena", [128, ARENA_BYTES // 2], BF16)
        self.A = self.view(0, [8, SEQ], BF16)
        self.B = self.view(65536, [8, SEQ], BF16)
        self.bA = [[Buf() for _ in range(NCH)] for _ in range(8)]
        self.bB = [[Buf() for _ in range(NCH)] for _ in range(8)]
        self.tabC = self.view(131072, [SEQ], BF16)
        self.tabS = self.view(131072 + 8192, [SEQ], BF16)
        self.b_tab = Buf()
        self.cstb = self.view(147456, [CB_N], BF16)
        o = 147456 + CB_N * 2
        o = (o + 63) // 64 * 64
        self.cstf = self.view(o, [CF_N], F32)
        o += CF_N * 4
        self.pcols = self.view(o, [DEPTH * NP], F32)
        o += DEPTH * NP * 4
        o = (o + 63) // 64 * 64
        self.b_cst = Buf()
        self.AR0 = o
        self.AR_END = ARENA_BYTES
        self.cqn = self.view(self.AR0, [2, SEQ], BF16)
        self.ckvn = self.view(self.AR0 + 16384, [SEQ], BF16)
        self.krf = self.view(self.AR0 + 24576, [SEQ], BF16)
        self.b_cqn = [Buf() for _ in range(NCH)]
        self.b_ckvn = [Buf() for _ in range(NCH)]
        self.b_kr = [Buf() for _ in range(NCH)]
        self.b_fs = Buf()
        self.AR1 = self.AR0 + 32768
        self.ps = [nc.alloc_psum_tensor("ps%d" % i, [128, 512], F32) for i in range(8)]
        self.bps = [Buf() for _ in range(8)]
        self.build()

    def view(self, off, shape, dtype):
        size = 2 if dtype == BF16 else 4
        n = int(np.prod(shape))
        assert off % 4 == 0
        v = self.arena[:, off // 2: off // 2 + n * size // 2]
        if dtype != BF16:
            v = v.bitcast(dtype)
        if len(shape) == 2:
            v = v.rearrange("p (a b) -> p a b", a=shape[0])
        elif len(shape) == 3:
            v = v.rearrange("p (a b c) -> p a b c", a=shape[0], b=shape[1])
        return v

    def pc(self, l, col, rows=slice(0, 128)):
        return self.pcols[rows, l * NP + col: l * NP + col + 1]

    def dbg_out(self, name, ap_sb, shape, bufs, dtype=F32):
        d = self.nc.dram_tensor("dbg_" + name, list(shape), dtype, kind="ExternalOutput").ap()
        o = self.S.op("sp", DMA(d, ap_sb), reads=bufs, writes=[Buf()], dma=True)
        self.out_ops.append(o)
        self.dbg_outs[name] = shape

    def build(self):
        S = self.S
        cs_all = [slice(c * TC, (c + 1) * TC) for c in range(NCH)]
        self.cs = cs_all
        S.op("pool", DMA(self.cstb[:, :], self.cstb_d), writes=[self.b_cst], dma=True)
        S.op("sp", DMA(self.cstf[:, :], self.cstf_d), writes=[self.b_cst], dma=True)
        S.op("sp", DMA(self.pcols[:, :], self.pcols_d), writes=[self.b_cst], dma=True)
        for dc in range(8):
            S.op("pool", DMA(self.A[:, dc, :], self.xT[dc]), writes=self.bA[dc], dma=True)
        self.precast()
        self.rope_tables()
        if self.dbg == "tab":
            self.dbg_out("tabC", self.tabC[:, :], [128, SEQ], [self.b_tab], BF16)
            self.dbg_out("tabS", self.tabS[:, :], [128, SEQ], [self.b_tab], BF16)
            return self.finish()
        for l in range(self.n_layers):
            if l % 2 == 0:
                X, M, bX, bM = self.A, self.B, self.bA, self.bB
            else:
                X, M, bX, bM = self.B, self.A, self.bB, self.bA
            self.X, self.M, self.bX, self.bM = X, M, bX, bM
            self.phase_p1(l)
            self.phase_p2a(l)
            if self.dbg == "p2a":
                S.barrier()
                self.dbg_out("cqn", self.cqn[:, :, :], [128, 2, SEQ], self.b_cqn, BF16)
                self.dbg_out("ckvn", self.ckvn[:, :], [128, SEQ], self.b_ckvn, BF16)
                self.dbg_out("krf", self.krf[:, :], [128, SEQ], self.b_kr + [self.b_fs], BF16)
                return self.finish()
            self.phase_conv(l)
            self.phase_fox(l)
            self.phase_mla(l)
            if self.dbg == "mix":
                S.barrier()
                self.dbg_out("mixed", M[:, :, :], [128, 8, SEQ], [b for r in bM for b in r], BF16)
                return self.finish()
            self.phase_post(l)
            if self.dbg == "l0":
                break
        self.finish()

    def precast(self):
        S = self.S

        def pc(key, dst, src):
            b = Buf()
            self.wb[key] = b
            S.op("pool", DMA(dst, src), writes=[b], dma=True, free=True)

        def attn(l):
            pc(("win", l), self.sWin[l], self.w_in[l].rearrange("(kc p) f -> p kc f", p=128))
            pc(("wuq", l), self.sWuq[l], self.w_uq[l].rearrange("(kc p) f -> p kc f", p=128))
            pc(("wukv", l), self.sWukv[l], self.w_ukv[l])
            pc(("wout", l), self.sWout[l], self.w_out[l].rearrange("(kc p) f -> p kc f", p=128))

        def dense(j):
            for e_ in range(2):
                for q, wsrc in ((0, self.dw1), (1, self.dw3)):
                    wv = wsrc[j].rearrange("(kc p) f -> p kc f", p=128)
                    for f in range(11):
                        c0 = e_ * 1408 + f * 128
                        pc(("d13", j, e_, q, f), self.sD13[j][e_][q][f], wv[:, :, c0:c0 + 128])
                wv = self.dw2[j][e_ * 1408:(e_ + 1) * 1408, :].rearrange("(f p) d -> p f d", p=128)
                for dc in range(8):
                    pc(("d2", j, e_, dc), self.sD2[j][e_][dc], wv[:, :, dc * 128:(dc + 1) * 128])

        def experts(j):
            for e_ in range(8):
                for q, wsrc in ((0, self.ew1), (1, self.ew3)):
                    wv = wsrc[j][e_].rearrange("(kc p) f -> p kc f", p=128)
                    for f in range(11):
                        pc(("e13", j, e_, q, f), self.sE13[j][e_][q][f], wv[:, :, f * 128:(f + 1) * 128])
                wv = self.ew2[j][e_].rearrange("(f p) d -> p f d", p=128)
                for dc in range(8):
                    pc(("e2", j, e_, dc), self.sE2[j][e_][dc], wv[:, :, dc * 128:(dc + 1) * 128])

        for l in range(self.n_layers):
            attn(l)
            if l % 2 == 0:
                dense(l // 2)
            else:
                experts(l // 2)

    def finish(self):
        self.S.emit(final_waits=self.out_ops + self.S.tagged)

    def rope_tables(self):
        S = self.S
        posI = self.view(65536, [SEQ], I32)
        ang = self.view(65536 + 16384, [SEQ], F32)
        r = self.view(65536 + 32768, [SEQ], F32)
        bb = Buf()
        R = slice(64, 96)
        S.op("sp", DMA(posI[R, :], self.posrep[R, :]), writes=[bb], dma=True)
        S.op("dve", CP(ang[R, :], posI[R, :]), reads=[bb], writes=[bb])
        S.op("dve", TS(ang[R, :], ang[R, :], self.cstf[R, CF_IF:CF_IF + 1], None, ALU.mult), reads=[bb, self.b_cst], writes=[bb])
        kq = self.view(65536 + 49152, [SEQ], F32)
        ki = self.view(65536 + 49152 + 16384 - 16384, [SEQ], F32)
        kI = posI

        def reduce_and_sin(shift, dst_fn):
            S.op("dve", TS(kq[R, :], ang[R, :], shift, 1.0 / (2 * PI), ALU.add, ALU.mult), reads=[bb], writes=[bb])
            S.op("dve", CP(kI[R, :], kq[R, :]), reads=[bb], writes=[bb])
            S.op("dve", CP(kq[R, :], kI[R, :]), reads=[bb], writes=[bb])
            S.op("dve", STT(r[R, :], kq[R, :], -2 * PI, ang[R, :], ALU.mult, ALU.add), reads=[bb], writes=[bb])
            if shift != 0.0:
                S.op("dve", TS(r[R, :], r[R, :], shift, None, ALU.add), reads=[bb], writes=[bb])
            S.op("dve", TS(kq[R, :], r[R, :], PI, 2 * PI, ALU.is_gt, ALU.mult), reads=[bb], writes=[bb])
            S.op("dve", TT(r[R, :], r[R, :], kq[R, :], ALU.subtract), reads=[bb], writes=[bb])
            S.op("dve", TS(kq[R, :], r[R, :], -PI, 2 * PI, ALU.is_lt, ALU.mult), reads=[bb], writes=[bb])
            S.op("dve", TT(r[R, :], r[R, :], kq[R, :], ALU.add), reads=[bb], writes=[bb])
            S.op("act", ACT(r[R, :], r[R, :], AF.Sin), reads=[bb], writes=[bb])
            dst_fn()

        reduce_and_sin(0.0, lambda: S.op("dve", TS(self.tabS[R, :], r[R, :], self.cstf[R, CF_NS:CF_NS + 1], None, ALU.mult),
                                         reads=[bb, self.b_cst], writes=[self.b_tab]))
        reduce_and_sin(PI / 2, lambda: S.op("dve", CP(self.tabC[R, :], r[R, :]), reads=[bb], writes=[self.b_tab]))
        S.barrier()

    def rstd_bcast(self, ssum_ps, bsum, inv_n, Rt, bR, tmp, btmp):
        S = self.S
        S.op("act", ACT(tmp, ssum_ps, AF.Ln, bias=EPS, scale=inv_n), reads=[bsum], writes=[btmp])
        S.op("act", ACT(Rt, tmp, AF.Exp, scale=-0.5), reads=[btmp], writes=[bR])

    def phase_p1(self, l):
        S, X, bX = self.S, self.X, self.bX
        S.barrier()
        w = self.view(self.AR1, [8, 384], BF16)
        bw = Buf()
        S.op("sp", DMA(w, self.sWin[l][:, :, 0:384]), reads=[self.wb[("win", l)]], writes=[bw], dma=True)
        o = self.AR1 + 6144
        sq = self.view(o, [3, TC], BF16); o += 3072
        Rq = self.view(o, [TC], F32); o += 2048
        Rk = self.view(o, [TC], F32); o += 2048
        t1 = self.view(o, [TC], F32); o += 2048
        t2 = self.view(o, [TC], F32); o += 2048
        b_sq, b_Rq, b_Rk, b_t1, b_t2 = Buf(), Buf(), Buf(), Buf(), Buf()
        ones = self.cstb[:, CB_ONE:CB_ONE + 128]
        for c in range(NCH):
            cs = self.cs[c]
            base = 3 * (c % 2)
            for k in range(3):
                p = base + k
                for dc in range(8):
                    S.op("pe", MM(self.ps[p][:, :], w[:, dc, k * 128:(k + 1) * 128], X[:, dc, cs], dc == 0, dc == 7),
                         reads=[bw, bX[dc][c]], writes=[self.bps[p]])
                S.op("act", ACT(sq[:, k, :], self.ps[p][:, :], AF.Square), reads=[self.bps[p]], writes=[b_sq])
            S.op("pe", MM(self.ps[6][:, :], ones, sq[:, 0, :], True, False), reads=[b_sq, self.b_cst], writes=[self.bps[6]])
            S.op("pe", MM(self.ps[6][:, :], ones, sq[:, 1, :], False, True), reads=[b_sq], writes=[self.bps[6]])
            S.op("pe", MM(self.ps[7][:, :], ones, sq[:, 2, :], True, True), reads=[b_sq], writes=[self.bps[7]])
            self.rstd_bcast(self.ps[6][:, :], self.bps[6], 1.0 / 256, Rq[:, :], b_Rq, t1[:, :], b_t1)
            self.rstd_bcast(self.ps[7][:, :], self.bps[7], 1.0 / 128, Rk[:, :], b_Rk, t2[:, :], b_t2)
            for k in range(2):
                S.op("dve", STT(self.cqn[:, k, cs], self.ps[base + k][:, :], self.pc(l, PC_QG + k), Rq[:, :], ALU.mult, ALU.mult),
                     reads=[self.bps[base + k], b_Rq, self.b_cst], writes=[self.b_cqn[c]])
            S.op("dve", STT(self.ckvn[:, cs], self.ps[base + 2][:, :], self.pc(l, PC_KVG), Rk[:, :], ALU.mult, ALU.mult),
                 reads=[self.bps[base + 2], b_Rk, self.b_cst], writes=[self.b_ckvn[c]])

    def phase_p2a(self, l):
        S, X, bX, M = self.S, self.X, self.bX, self.M
        S.barrier()
        w = self.view(self.AR1, [8, 320], BF16)
        bw = Buf()
        S.op("sp", DMA(w, self.sWin[l][:, :, 384:704]), reads=[self.wb[("win", l)]], writes=[bw], dma=True)
        o = self.AR1 + 5120
        t1 = self.view(o, [TC], F32); o += 2048
        t2 = self.view(o, [TC], F32); o += 2048
        z = self.view(o, [TC], F32); o += 2048
        b_t1, b_t2, b_z = Buf(), Buf(), Buf()
        mbase = 0 if M is self.A else 65536
        Fraw = self.view(mbase, [SEQ], F32)
        Fc = self.view(mbase + 16384, [SEQ], F32)
        b_Fraw, b_Fc = Buf(), Buf()
        S.op("dve", MEMSET(self.krf[:, :], 1.0), writes=self.b_kr + [self.b_fs])
        R = slice(64, 96)
        G = slice(0, 64)
        for c in range(NCH):
            cs = self.cs[c]
            base = 3 * (c % 2)
            for k, (c0, c1) in enumerate(((0, 96), (96, 192), (192, 320))):
                p = base + k
                m = c1 - c0
                for dc in range(8):
                    S.op("pe", MM(self.ps[p][0:m, :], w[:, dc, c0:c1], X[:, dc, cs], dc == 0, dc == 7),
                         reads=[bw, bX[dc][c]], writes=[self.bps[p]])
            S.op("dve", TT(t1[R, :], self.ps[base][R, :], self.tabC[R, cs], ALU.mult), reads=[self.bps[base], self.b_tab], writes=[b_t1])
            S.op("dve", TT(t2[R, :], self.ps[base + 1][R, :], self.tabS[R, cs], ALU.mult), reads=[self.bps[base + 1], self.b_tab], writes=[b_t2])
            S.op("dve", TT(self.krf[R, cs], t1[R, :], t2[R, :], ALU.add), reads=[b_t1, b_t2], writes=[self.b_kr[c]])
            S.op("dve", TS(z[G, :], self.ps[base + 2][G, :], self.pc(l, PC_FB, G), -80.0, ALU.add, ALU.max),
                 reads=[self.bps[base + 2], self.b_cst], writes=[b_z])
            S.op("act", ACT(z[G, :], z[G, :], AF.Exp, scale=-1.0), reads=[b_z], writes=[b_z])
            S.op("act", ACT(Fraw[G, cs], z[G, :], AF.Ln, bias=1.0), reads=[b_z], writes=[b_Fraw])
        S.op("dve", lambda e: e.tensor_tensor_scan(out=Fc[G, :], data0=Fraw[G, :], data1=Fraw[G, :], initial=0.0,
                                                   op0=ALU.add, op1=ALU.bypass), reads=[b_Fraw], writes=[b_Fc])
        S.op("dve", TS(Fc[G, :], Fc[G, :], -1.0, None, ALU.mult), reads=[b_Fc], writes=[b_Fc])
        hi = self.view(mbase, [SEQ], BF16)
        b_hi = Buf()
        S.op("dve", CP(hi[G, :], Fc[G, :]), reads=[b_Fc, b_Fraw], writes=[b_hi, b_Fraw])
        S.op("dve", TT(Fc[G, :], Fc[G, :], hi[G, :], ALU.subtract), reads=[b_Fc, b_hi], writes=[b_Fc])
        S.op("dve", CP(self.krf[0:32, :], hi[0:32, :]), reads=[b_hi], writes=[self.b_fs])
        S.op("dve", CP(self.krf[32:64, :], Fc[32:64, :]), reads=[b_Fc], writes=[self.b_fs])

    def phase_conv(self, l):
        S, X, bX, M, bM = self.S, self.X, self.bX, self.M, self.bM
        S.barrier()
        w = self.view(self.AR1, [8, 512], BF16)
        bw = Buf()
        S.op("sp", DMA(w, self.sWin[l][:, :, 704:1216]), reads=[self.wb[("win", l)]], writes=[bw], dma=True)
        o = self.AR1 + 8192
        acc = self.view(o, [TC], F32); o += 2048
        eg = self.view(o, [TC], F32); o += 2048
        ab = self.view(o, [2, TC], BF16); o += 2048
        mm_ = self.view(o, [TC], F32); o += 2048
        rs = self.view(o, [TC], F32); o += 2048
        assert o <= self.AR_END
        b_acc, b_eg, b_ab, b_mm, b_rs = Buf(), Buf(), Buf(), Buf(), Buf()
        mbase = 0 if M is self.A else 65536
        HW = 30 + TC
        hb = [[self.view(mbase + (s * 2 + cc) * 1088, [HW], BF16) for cc in range(2)] for s in range(2)]
        b_hb = [[Buf(), Buf()], [Buf(), Buf()]]
        Dm = self.view(mbase + 8704, [62, 128], BF16)
        b_D = Buf()
        ident = self.cstb[:, CB_ID:CB_ID + 128]
        for j in range(31):
            for cc in range(2):
                S.op("dve", TS(Dm[:, j * 2 + cc, :], ident, self.pc(l, PC_CW + j * 2 + cc), None, ALU.mult),
                     reads=[self.b_cst], writes=[b_D])
        BD = self.cstb[:, CB_BD:CB_BD + 128]
        for c in range(NCH):
            cs = self.cs[c]
            s = c % 2
            for cc in range(2):
                pa, pg, pc_ = 0 + 2 * cc, 1 + 2 * cc, 4 + cc
                h = hb[s][cc]; bh = b_hb[s][cc]
                if c == 0:
                    S.op("dve", MEMSET(h[:, 0:30], 0.0), writes=[bh])
                else:
                    S.op("dve", CP(h[:, 0:30], hb[1 - s][cc][:, TC:TC + 30]), reads=[b_hb[1 - s][cc]], writes=[bh])
                for dc in range(8):
                    S.op("pe", MM(self.ps[pa][:, :], w[:, dc, cc * 128:(cc + 1) * 128], X[:, dc, cs], dc == 0, dc == 7),
                         reads=[bw, bX[dc][c]], writes=[self.bps[pa]])
                for dc in range(8):
                    S.op("pe", MM(self.ps[pg][:, :], w[:, dc, 256 + cc * 128:256 + (cc + 1) * 128], X[:, dc, cs], dc == 0, dc == 7),
                         reads=[bw, bX[dc][c]], writes=[self.bps[pg]])
                S.op("act", ACT(eg[:, :], self.ps[pg][:, :], AF.Exp, scale=-1.0), reads=[self.bps[pg]], writes=[b_eg])
                S.op("act", ACT(eg[:, :], eg[:, :], AF.Ln, bias=1.0), reads=[b_eg], writes=[b_eg])
                S.op("act", ACT(eg[:, :], eg[:, :], AF.Exp, scale=-1.0), reads=[b_eg], writes=[b_eg])
                S.op("dve", TT(h[:, 30:HW], self.ps[pa][:, :], eg[:, :], ALU.mult), reads=[self.bps[pa], b_eg], writes=[bh])
                for j in range(31):
                    S.op("pe", MM(self.ps[pc_][:, :], Dm[:, j * 2 + cc, :], h[:, j:j + TC], j == 0, j == 30),
                         reads=[b_D, bh], writes=[self.bps[pc_]])
                S.op("dve", TS(acc[:, :], self.ps[pc_][:, :], self.pc(l, PC_CB + cc), None, ALU.add),
                     reads=[self.bps[pc_], self.b_cst], writes=[b_acc])
                S.op("act", ACT(ab[:, 0, :], acc[:, :], AF.Copy), reads=[b_acc], writes=[b_ab])
                S.op("act", ACT(ab[:, 1, :], acc[:, :], AF.Square), reads=[b_acc], writes=[b_ab])
                S.op("pe", MM(self.ps[6][:, :], BD, ab[:, 0, :], True, True), reads=[b_ab, self.b_cst], writes=[self.bps[6]])
                S.op("pe", MM(self.ps[7][:, :], BD, ab[:, 1, :], True, True), reads=[b_ab], writes=[self.bps[7]])
                S.op("dve", CP(mm_[:, :], self.ps[6][:, :]), reads=[self.bps[6]], writes=[b_mm])
                S.op("dve", TT(rs[:, :], mm_[:, :], mm_[:, :], ALU.mult), reads=[b_mm], writes=[b_rs])
                S.op("dve", TT(rs[:, :], self.ps[7][:, :], rs[:, :], ALU.subtract), reads=[self.bps[7], b_rs], writes=[b_rs])
                S.op("act", ACT(rs[:, :], rs[:, :], AF.Ln, bias=EPS), reads=[b_rs], writes=[b_rs])
                S.op("act", ACT(rs[:, :], rs[:, :], AF.Exp, scale=-0.5), reads=[b_rs], writes=[b_rs])
                S.op("dve", TT(acc[:, :], acc[:, :], mm_[:, :], ALU.subtract), reads=[b_acc, b_mm], writes=[b_acc])
                S.op("dve", TT(acc[:, :], acc[:, :], rs[:, :], ALU.mult), reads=[b_acc, b_rs], writes=[b_acc])
                S.op("dve", TS(acc[:, :], acc[:, :], self.pc(l, PC_CNG + cc), self.pc(l, PC_CNB + cc), ALU.mult, ALU.add),
                     reads=[b_acc, self.b_cst], writes=[b_acc])
                S.op("act", ACT(eg[:, :], acc[:, :], AF.Exp, scale=-1.0), reads=[b_acc], writes=[b_eg])
                S.op("act", ACT(eg[:, :], eg[:, :], AF.Ln, bias=1.0), reads=[b_eg], writes=[b_eg])
                S.op("act", ACT(eg[:, :], eg[:, :], AF.Exp, scale=-1.0), reads=[b_eg], writes=[b_eg])
                S.op("dve", TT(M[:, 6 + cc, cs], acc[:, :], eg[:, :], ALU.mult), reads=[b_acc, b_eg], writes=[bM[6 + cc][c]])

    def attention(self, Q, bQ, K, bK, V, bV, KD, scale, odd, out_ap_fn, out_bufs, pT, b_pT, rsb, b_rsb, bcs, b_bcs,
                  hook=None, rhl=None):
        S = self.S
        ident = self.cstb[:, CB_ID:CB_ID + 128]
        negm = self.cstb[:, CB_NM:CB_NM + 128]
        MV = 128 if odd else 65
        blocks = [(c, j) for c in range(NCH) for j in range(4 * c + 4)]

        def qk(i):
            c, j = blocks[i]
            m = j - 4 * c
            off = 128 * m if m > 0 else 0
            p = i % 3
            diag = m >= 0
            S.op("pe", MM(self.ps[p][:, off:TC], K[0:KD, j * 128:(j + 1) * 128], Q[0:KD, c * TC + off:(c + 1) * TC], True, not diag),
                 reads=[bK[j // 4], bQ[c]], writes=[self.bps[p]])
            if diag:
                S.op("pe", MM(self.ps[p][:, off:off + 128], ident, negm, False, True), reads=[self.b_cst], writes=[self.bps[p]])

        pending = []

        def finish_fn(c, oa):
            rrow = slice(0, 1) if odd else slice(64, 65)
            orow = slice(64, 128) if odd else slice(0, 64)

            def fn():
                onesb = self.cstb[rrow, CB_ONE:CB_ONE + 128]
                S.op("pe", MM(self.ps[5][:, :], onesb, rhl[rrow, 0, :], True, False),
                     reads=[b_rsb, self.b_cst], writes=[self.bps[5]])
                S.op("pe", MM(self.ps[5][:, :], onesb, rhl[rrow, 1, :], False, True),
                     reads=[b_rsb], writes=[self.bps[5]])
                S.op("act", ACT(bcs[orow, :], self.ps[5][orow, :], AF.Exp, scale=-1.0), reads=[self.bps[5]], writes=[b_bcs])
                S.op("dve", TT(out_ap_fn(orow, c), self.ps[oa][orow, :], bcs[orow, :], ALU.mult),
                     reads=[self.bps[oa], b_bcs], writes=[out_bufs[c]])
            return fn

        qk(0)
        qk(1)
        for i, (c, j) in enumerate(blocks):
            while pending and pending[0][0] <= i:
                pending.pop(0)[1]()
            m = j - 4 * c
            off = 128 * m if m > 0 else 0
            p = i % 3
            S.op("act", ACT(pT[p][:, off:TC], self.ps[p][:, off:TC], AF.Exp, scale=scale), reads=[self.bps[p]], writes=[b_pT[p]])
            if i + 2 < len(blocks):
                qk(i + 2)
            oa = 3 + (c % 2)
            last = (j == 4 * c + 3)
            S.op("pe", MM(self.ps[oa][0:MV, off:TC], V[:, j, 0:MV], pT[p][:, off:TC], j == 0, last),
                 reads=[bV[j // 4], b_pT[p]], writes=[self.bps[oa]])
            if last:
                rrow = slice(0, 1) if odd else slice(64, 65)
                S.op("act", ACT(rsb[rrow, :], self.ps[oa][rrow, :], AF.Ln), reads=[self.bps[oa]], writes=[b_rsb])
                S.op("act", ACT(rhl[rrow, 0, :], rsb[rrow, :], AF.Copy), reads=[b_rsb], writes=[b_rsb])
                S.op("dve", TT(rsb[rrow, :], rsb[rrow, :], rhl[rrow, 0, :], ALU.subtract), reads=[b_rsb], writes=[b_rsb])
                S.op("dve", CP(rhl[rrow, 1, :], rsb[rrow, :]), reads=[b_rsb], writes=[b_rsb])
                if hook is not None:
                    hook(c)
                pending.append((i + 4, finish_fn(c, oa)))
        while pending:
            pending.pop(0)[1]()

    def phase_fox(self, l):
        S, X, bX, M, bM = self.S, self.X, self.bX, self.M, self.bM
        S.barrier()
        w = self.view(self.AR1, [8, 768], BF16)
        bw = Buf()
        S.op("sp", DMA(w, self.sWin[l][:, :, 1216:1984]), reads=[self.wb[("win", l)]], writes=[bw], dma=True)
        o = self.AR1 + 12288
        pT = []
        for i in range(3):
            pT.append(self.view(o, [TC], BF16)); o += 1024
        rsb = self.view(o, [TC], F32); o += 2048
        bcs = self.view(o, [TC], F32); o += 2048
        rhl = self.view(o, [2, TC], BF16); o += 2048
        assert o <= self.AR_END
        b_pT = [Buf(), Buf(), Buf()]
        b_rsb, b_bcs = Buf(), Buf()
        Q = M[:, 0, :]
        K = M[:, 1, :]
        V = M[:, 2, :].rearrange("p (t v) -> p t v", v=128)
        bQ, bK, bV = bM[0], bM[1], bM[2]
        for h in range(4):
            odd = h % 2 == 1
            S.op("dve", MEMSET(M[:, 2, :], 0.0), writes=bV)
            oc = 0 if odd else 64
            S.op("dve", MEMSET(V[:, :, oc:oc + 1], 1.0), writes=bV)
            vo = 64 if odd else 0
            selq = self.cstb[:, CB_SQ + h * 68: CB_SQ + (h + 1) * 68]
            selk = self.cstb[:, CB_SK + h * 68: CB_SK + (h + 1) * 68]
            for c in range(NCH):
                cs = self.cs[c]
                for which, (sel, col0, dst, bd) in enumerate(((selq, 0, Q, bQ), (selk, 256, K, bK))):
                    p = 5 + which
                    S.op("pe", MM(self.ps[p][0:68, :], sel, self.krf[:, cs], True, False),
                         reads=[self.b_cst, self.b_kr[c], self.b_fs], writes=[self.bps[p]])
                    for dc in range(8):
                        S.op("pe", MM(self.ps[p][0:64, :], w[:, dc, col0 + h * 64: col0 + (h + 1) * 64], X[:, dc, cs], False, dc == 7),
                             reads=[bw, bX[dc][c]], writes=[self.bps[p]])
                    S.op("act", ACT(dst[0:68, cs], self.ps[p][0:68, :], AF.Copy), reads=[self.bps[p]], writes=[bd[c]])
                p = 7
                for i in range(4):
                    tb = 4 * c + i
                    for dc in range(8):
                        S.op("pe", MM(self.ps[p][:, i * 64:(i + 1) * 64], X[:, dc, tb * 128:(tb + 1) * 128],
                                      w[:, dc, 512 + h * 64: 512 + (h + 1) * 64], dc == 0, dc == 7),
                             reads=[bw, bX[dc][c]], writes=[self.bps[p]])
                S.op("dve", CP(V[:, 4 * c:4 * c + 4, vo:vo + 64], self.ps[p][:, 0:256].rearrange("p (t v) -> p t v", v=64)),
                     reads=[self.bps[p]], writes=[bV[c]])
            kk = 4 + h // 2
            self.attention(Q, bQ, K, bK, V, bV, 68, 0.125, odd,
                           lambda orow, c, kk=kk: M[orow, kk, self.cs[c]], bM[kk], pT, b_pT, rsb, b_rsb, bcs, b_bcs, rhl=rhl)

    def mla_proj(self, l, h, c, wuq, wukv, bw, t1, t2, b_t1, b_t2):
        S, X, bX = self.S, self.X, self.bX
        s = h % 2
        odd = h % 2 == 1
        Q = X[:, s, :]; K = X[:, 2 + s, :]
        V = X[:, 4 + s, :].rearrange("p (t v) -> p t v", v=128)
        cs = self.cs[c]
        R = slice(64, 96)
        for k in range(2):
            S.op("pe", MM(self.ps[5][0:96, :], wuq[:, k, h * 96:(h + 1) * 96], self.cqn[:, k, cs], k == 0, k == 1),
                 reads=[bw, self.b_cqn[c]], writes=[self.bps[5]])
        for k in range(2):
            S.op("pe", MM(self.ps[6][0:96, :], wuq[:, k, 768 + h * 96:768 + (h + 1) * 96], self.cqn[:, k, cs], k == 0, k == 1),
                 reads=[bw, self.b_cqn[c]], writes=[self.bps[6]])
        S.op("act", ACT(Q[0:64, cs], self.ps[5][0:64, :], AF.Copy), reads=[self.bps[5]], writes=[bX[s][c]])
        S.op("dve", TT(t1[R, :], self.ps[5][R, :], self.tabC[R, cs], ALU.mult), reads=[self.bps[5], self.b_tab], writes=[b_t1])
        S.op("dve", TT(t2[R, :], self.ps[6][R, :], self.tabS[R, cs], ALU.mult), reads=[self.bps[6], self.b_tab], writes=[b_t2])
        S.op("dve", TT(Q[R, cs], t1[R, :], t2[R, :], ALU.add), reads=[b_t1, b_t2], writes=[bX[s][c]])
        S.op("pe", MM(self.ps[7][0:64, :], wukv[:, h * 128:h * 128 + 64], self.ckvn[:, cs], True, True),
             reads=[bw, self.b_ckvn[c]], writes=[self.bps[7]])
        S.op("act", ACT(K[0:64, cs], self.ps[7][0:64, :], AF.Copy), reads=[self.bps[7]], writes=[bX[2 + s][c]])
        S.op("dve", CP(K[R, cs], self.krf[R, cs]), reads=[self.b_kr[c]], writes=[bX[2 + s][c]])
        vo = 64 if odd else 0
        oc = 0 if odd else 64
        S.op("dve", MEMSET(V[:, 4 * c:4 * c + 4, :], 0.0), writes=[bX[4 + s][c]])
        S.op("dve", MEMSET(V[:, 4 * c:4 * c + 4, oc:oc + 1], 1.0), writes=[bX[4 + s][c]])
        for i in range(4):
            tb = 4 * c + i
            S.op("pe", MM(self.ps[7][:, 256 + i * 64:256 + (i + 1) * 64], self.ckvn[:, tb * 128:(tb + 1) * 128],
                          wukv[:, h * 128 + 64:h * 128 + 128], True, True),
                 reads=[bw, self.b_ckvn[c]], writes=[self.bps[7]])
        S.op("dve", CP(V[:, 4 * c:4 * c + 4, vo:vo + 64], self.ps[7][:, 256:512].rearrange("p (t v) -> p t v", v=64)),
             reads=[self.bps[7]], writes=[bX[4 + s][c]])

    def phase_mla(self, l):
        S, X, bX, M, bM = self.S, self.X, self.bX, self.M, self.bM
        S.barrier()
        o = self.AR1
        wuq = self.view(o, [2, 1536], BF16); o += 6144
        wukv = self.view(o, [1024], BF16); o += 2048
        bw = Buf()
        S.op("sp", DMA(wuq, self.sWuq[l]), reads=[self.wb[("wuq", l)]], writes=[bw], dma=True)
        S.op("sp", DMA(wukv, self.sWukv[l]), reads=[self.wb[("wukv", l)]], writes=[bw], dma=True)
        pT = []
        for i in range(3):
            pT.append(self.view(o, [TC], BF16)); o += 1024
        rsb = self.view(o, [TC], F32); o += 2048
        bcs = self.view(o, [TC], F32); o += 2048
        t1 = self.view(o, [TC], F32); o += 2048
        t2 = self.view(o, [TC], F32); o += 2048
        rhl = self.view(o, [2, TC], BF16); o += 2048
        assert o <= self.AR_END
        b_pT = [Buf(), Buf(), Buf()]
        b_rsb, b_bcs, b_t1, b_t2 = Buf(), Buf(), Buf(), Buf()
        scale = 96.0 ** -0.5
        for c in range(NCH):
            self.mla_proj(l, 0, c, wuq, wukv, bw, t1, t2, b_t1, b_t2)
        for h in range(8):
            s = h % 2
            odd = h % 2 == 1
            Q = X[:, s, :]; K = X[:, 2 + s, :]
            V = X[:, 4 + s, :].rearrange("p (t v) -> p t v", v=128)
            kk = h // 2
            hook = None
            if h + 1 < 8:
                hook = (lambda c, h=h: self.mla_proj(l, h + 1, c, wuq, wukv, bw, t1, t2, b_t1, b_t2))
            self.attention(Q, bX[s], K, bX[2 + s], V, bX[4 + s], 96, scale, odd,
                           lambda orow, c, kk=kk: M[orow, kk, self.cs[c]], bM[kk], pT, b_pT, rsb, b_rsb, bcs, b_bcs, hook=hook, rhl=rhl)

    def ln_stats_add(self, ydc, by, dc, yb, b_yb):
        S = self.S
        ones = self.cstb[:, CB_ONE:CB_ONE + 128]
        i = dc % 2
        S.op("act", ACT(yb[i][:, 0, :], ydc, AF.Copy), reads=[by], writes=[b_yb[i]])
        S.op("act", ACT(yb[i][:, 1, :], ydc, AF.Square), reads=[by], writes=[b_yb[i]])
        S.op("pe", MM(self.ps[4][:, :], ones, yb[i][:, 0, :], dc == 0, dc == 7), reads=[b_yb[i], self.b_cst], writes=[self.bps[4]])
        S.op("pe", MM(self.ps[5][:, :], ones, yb[i][:, 1, :], dc == 0, dc == 7), reads=[b_yb[i]], writes=[self.bps[5]])

    def ln_finalize(self, l, ybuf, b_y, gcol, bcol, tmp, b_tmp, lt, b_lt, bf_dst_fn, bf_bufs_fn, f32_hook=None):
        S = self.S
        m, msq, rstd, nmr = tmp
        b_m, b_msq, b_rstd, b_nmr = b_tmp
        S.op("dve", TS(m, self.ps[4][:, :], 1.0 / D, None, ALU.mult), reads=[self.bps[4]], writes=[b_m])
        S.op("dve", TT(msq, m, m, ALU.mult), reads=[b_m], writes=[b_msq])
        S.op("dve", STT(msq, self.ps[5][:, :], 1.0 / D, msq, ALU.mult, ALU.subtract), reads=[self.bps[5], b_msq], writes=[b_msq])
        S.op("act", ACT(msq, msq, AF.Ln, bias=EPS), reads=[b_msq], writes=[b_msq])
        S.op("act", ACT(rstd, msq, AF.Exp, scale=-0.5), reads=[b_msq], writes=[b_rstd])
        S.op("dve", TT(nmr, m, rstd, ALU.mult), reads=[b_m, b_rstd], writes=[b_nmr])
        for dc in range(8):
            i = dc % 2
            S.op("dve", TT(lt[i], ybuf[:, dc, :], rstd, ALU.mult), reads=[b_y[dc], b_rstd], writes=[b_lt[i]])
            S.op("dve", TT(lt[i], lt[i], nmr, ALU.subtract), reads=[b_lt[i], b_nmr], writes=[b_lt[i]])
            S.op("act", ACT(ybuf[:, dc, :], lt[i], AF.Identity, bias=self.pc(l, bcol + dc), scale=self.pc(l, gcol + dc)),
                 reads=[b_lt[i], self.b_cst], writes=[b_y[dc]])
            S.op("act", ACT(bf_dst_fn(dc), ybuf[:, dc, :], AF.Copy), reads=[b_y[dc]], writes=bf_bufs_fn(dc))
            if f32_hook is not None:
                f32_hook(dc)

    def phase_post(self, l):
        S, X, bX, M, bM = self.S, self.X, self.bX, self.M, self.bM
        S.barrier()
        j = l // 2
        moe = (l % 2 == 1)
        last_layer = (l == DEPTH - 1)
        xbase = 0 if X is self.A else 65536
        NB = 2
        NF = 11
        o = xbase
        wout = self.view(o, [8, D], BF16); o += 16384
        ybufs = []
        for i in range(NB):
            ybufs.append(self.view(o, [8, TC], F32)); o += 16384
        x1bs = []
        for i in range(NB):
            x1bs.append(self.view(o, [8, TC], BF16)); o += 8192
        assert o <= xbase + 65536
        o = self.AR0
        nxr = 1
        xr = [self.view(o + i * 2048, [TC], F32) for i in range(nxr)]; o += 2048 * nxr
        yb = [self.view(o + i * 2048, [2, TC], BF16) for i in range(2)]; o += 4096
        tmp = [self.view(o + i * 2048, [TC], F32)[:, :] for i in range(4)]; o += 8192
        lt = [self.view(o + i * 2048, [TC], F32)[:, :] for i in range(2)]; o += 4096
        sa = [self.view(o + i * 2048, [TC], F32) for i in range(2)]; o += 4096
        gw = 128
        w13 = [[self.view(o + (s_ * 2 + q) * gw * 16, [8, gw], BF16) for q in range(2)] for s_ in range(2)]; o += 4 * gw * 16
        w2d = [self.view(o + s_ * (NF * 256), [NF, 128], BF16) for s_ in range(2)]; o += 2 * NF * 256
        gb = lg = sm = rwt = None
        gTs = [None, None]
        hT = self.view(o, [NF, TC], BF16); o += NF * 1024
        if moe:
            gb = self.view(o, [TC], F32); o += 2048
            gTs = [self.view(o + i * 2048, [TC], F32) for i in range(2)]; o += 4096
            lg = self.view(o, [4, 8], F32); o += 128
            sm = self.view(o, [64], F32); o += 256
            rwt = self.view(o, [8, 8], F32); o += 256
        assert o <= self.AR_END, o
        b_wout = Buf()
        b_x1bs = [Buf() for _ in range(NB)]
        b_ys = [[Buf() for _ in range(8)] for _ in range(NB)]
        b_xr, b_yb, b_lt, b_sa = [Buf(), Buf()], [Buf(), Buf()], [Buf(), Buf()], [Buf(), Buf()]
        b_tmp = [Buf() for _ in range(4)]
        b_h = [Buf() for _ in range(NF)]
        b_w13 = [[Buf(), Buf()], [Buf(), Buf()]]
        b_w2d = [Buf(), Buf()]
        b_gb, b_lg, b_sm, b_rw = Buf(), Buf(), Buf(), Buf()
        b_gTs = [Buf(), Buf()]
        ones = self.cstb[:, CB_ONE:CB_ONE + 128]
        S.op("sp", DMA(wout, self.sWout[l]), reads=[self.wb[("wout", l)]], writes=[b_wout], dma=True)
        if moe:
            S.op("sp", DMA(rwt, self.rw[j].rearrange("(kc p) e -> p kc e", p=128)), writes=[b_rw], dma=True)
        wcnt = [0, 0]

        def pre_stages(c):
            cs = self.cs[c]
            ybuf, b_y, x1b, b_x1b = ybufs[c % NB], b_ys[c % NB], x1bs[c % NB], b_x1bs[c % NB]

            def rms_group(k0, nk, gcol):
                def fn():
                    for k in range(nk):
                        i = k % 2
                        S.op("act", ACT(yb[i][:, 0, :], M[:, k0 + k, cs], AF.Square), reads=[bM[k0 + k][c]], writes=[b_yb[i]])
                        S.op("pe", MM(self.ps[6][:, :], ones, yb[i][:, 0, :], k == 0, k == nk - 1), reads=[b_yb[i], self.b_cst], writes=[self.bps[6]])
                    S.op("act", ACT(tmp[0], self.ps[6][:, :], AF.Ln, bias=EPS, scale=1.0 / (nk * 128)), reads=[self.bps[6]], writes=[b_tmp[0]])
                    S.op("act", ACT(tmp[1], tmp[0], AF.Exp, scale=-0.5), reads=[b_tmp[0]], writes=[b_tmp[1]])
                    for k in range(nk):
                        S.op("dve", STT(M[:, k0 + k, cs], M[:, k0 + k, cs], self.pc(l, gcol + k), tmp[1], ALU.mult, ALU.mult),
                             reads=[bM[k0 + k][c], b_tmp[1], self.b_cst], writes=[bM[k0 + k][c]])
                return fn

            def wout_part(dcs):
                def fn():
                    for dc in dcs:
                        i = dc % nxr
                        p = 6
                        src = self.xT[dc][:, cs] if l == 0 else self.xres[dc][:, cs]
                        rd = [] if l == 0 else [self.b_xres[dc][c]]
                        S.op("sp", DMA(xr[i][:, :], src), reads=rd, writes=[b_xr[i]], dma=True)
                        for k in range(8):
                            S.op("pe", MM(self.ps[p][:, :], wout[:, k, dc * 128:(dc + 1) * 128], M[:, k, cs], k == 0, k == 7),
                                 reads=[b_wout, bM[k][c]], writes=[self.bps[p]])
                        S.op("dve", STT(ybuf[:, dc, :], xr[i][:, :], ALPHA, self.ps[p][:, :], ALU.mult, ALU.add),
                             reads=[b_xr[i], self.bps[p]], writes=[b_y[dc]])
                        self.ln_stats_add(ybuf[:, dc, :], b_y[dc], dc, yb, b_yb)
                return fn

            def ln1():
                self.ln_finalize(l, ybuf, b_y, PC_L1G, PC_L1B, tmp, b_tmp, lt, b_lt,
                                 lambda dc: x1b[:, dc, :], lambda dc: [b_x1b])

            st = [rms_group(0, 4, PC_MOG), rms_group(4, 2, PC_FOG), wout_part(range(0, 4)), wout_part(range(4, 8)), ln1]
            def scale_y():
                for dc in range(8):
                    S.op("dve", TS(ybuf[:, dc, :], ybuf[:, dc, :], ALPHA, None, ALU.mult), reads=[b_y[dc]], writes=[b_y[dc]])

            if moe:
                st.append(lambda: self.moe_router_a(ybuf, b_y, gTs[c % 2], b_gTs[c % 2], lg, b_lg, rwt, b_rw))
                st.append(lambda: self.moe_router_b(ybuf, b_y, gTs[c % 2], b_gTs[c % 2], lg, b_lg, sm, b_sm))
            else:
                st.append(scale_y)
            return st

        def pre(c):
            for f in pre_stages(c):
                f()

        def post(c):
            cs = self.cs[c]
            ybuf, b_y = ybufs[c % NB], b_ys[c % NB]
            for dc in range(8):
                self.ln_stats_add(ybuf[:, dc, :], b_y[dc], dc, yb, b_yb)

            def f32_out(dc):
                if last_layer or self.dbg == "l0":
                    S.op("sp", DMA(self.outT[dc][:, cs], ybuf[:, dc, :]), reads=[b_y[dc]], writes=[self.b_out], dma=True, tag="out")
                else:
                    S.op("sp", DMA(self.xres[dc][:, cs], ybuf[:, dc, :]), reads=[b_y[dc]], writes=[self.b_xres[dc][c]], dma=True)

            self.ln_finalize(l, ybuf, b_y, PC_L2G, PC_L2B, tmp, b_tmp, lt, b_lt,
                             lambda dc: M[:, dc, cs], lambda dc: [bM[dc][c]], f32_hook=f32_out)

        n_exp = 8 if moe else 2
        pre(0)
        for c in range(NCH):
            def bgwork(c=c):
                if c >= 1:
                    post(c - 1)
                if c + 1 < NCH:
                    pre(c + 1)
            bg = S.capture(bgwork)
            nsteps = [n_exp * 19]
            tail = 24 if moe else 6

            def step(bg=bg, nsteps=nsteps, tail=tail):
                if bg:
                    n = -(-len(bg) // max(nsteps[0] - tail, 1))
                    S.replay(bg, n)
                nsteps[0] -= 1

            self.moe_experts(j, ybufs[c % 2], b_ys[c % 2], x1bs[c % 2], b_x1bs[c % 2], hT, b_h, w13, b_w13, w2d, b_w2d,
                             sa, b_sa, wcnt, gb, b_gb, gTs[c % 2], b_gTs[c % 2], step=step, moe=moe)
            S.replay(bg, len(bg))
        post(NCH - 1)

    def ffn_dense(self, j, ybuf, b_y, x1b, b_x1b, hT, b_h, w13, b_w13, w2d, b_w2d, sa, b_sa, wcnt):
        S = self.S
        w1v = self.dw1[j].rearrange("(kc p) f -> p kc f", p=128)
        w3v = self.dw3[j].rearrange("(kc p) f -> p kc f", p=128)
        for g in range(11):
            s = wcnt[0] % 2; wcnt[0] += 1
            S.op("sp", DMA(w13[s][0], self.sD13[j][0][g]), reads=[self.wb[("d13", j, 0, g)]], writes=[b_w13[s][0]], dma=True)
            S.op("sp", DMA(w13[s][1], self.sD13[j][1][g]), reads=[self.wb[("d13", j, 1, g)]], writes=[b_w13[s][1]], dma=True)
            for a in range(2):
                f = 2 * g + a
                pa, pb = (0, 1) if f % 2 == 0 else (2, 3)
                for q, p in ((0, pa), (1, pb)):
                    for dc in range(8):
                        S.op("pe", MM(self.ps[p][:, :], w13[s][q][:, dc, a * 128:(a + 1) * 128], x1b[:, dc, :], dc == 0, dc == 7),
                             reads=[b_w13[s][q], b_x1b], writes=[self.bps[p]])
                i = f % 2
                S.op("act", ACT(sa[i][:, :], self.ps[pa][:, :], AF.Silu), reads=[self.bps[pa]], writes=[b_sa[i]])
                S.op("dve", TT(hT[:, f, :], sa[i][:, :], self.ps[pb][:, :], ALU.mult), reads=[b_sa[i], self.bps[pb]], writes=[b_h[f]])
        for dc in range(8):
            s = wcnt[1] % 2; wcnt[1] += 1
            S.op("sp", DMA(w2d[s], self.sD2[j][dc]), reads=[self.wb[("d2", j, dc)]], writes=[b_w2d[s]], dma=True)
            p = dc % 4
            for f in range(22):
                S.op("pe", MM(self.ps[p][:, :], w2d[s][:, f, :], hT[:, f, :], f == 0, f == 21),
                     reads=[b_w2d[s], b_h[f]], writes=[self.bps[p]])
            S.op("dve", STT(ybuf[:, dc, :], ybuf[:, dc, :], ALPHA, self.ps[p][:, :], ALU.mult, ALU.add),
                 reads=[b_y[dc], self.bps[p]], writes=[b_y[dc]])

    def moe_router_a(self, ybuf, b_y, gT, b_gT, lg, b_lg, rwt, b_rw):
        S = self.S
        identf = self.cstf[:, CF_ID:CF_ID + 128]
        for dc in range(8):
            S.op("pe", MM(self.ps[6][0:8, :], rwt[:, dc, :], ybuf[:, dc, :], dc == 0, dc == 7),
                 reads=[b_rw, b_y[dc]], writes=[self.bps[6]])
        S.op("act", ACT(gT[0:8, :], self.ps[6][0:8, :], AF.Copy), reads=[self.bps[6]], writes=[b_gT])
        for b in range(4):
            S.op("pe", MM(self.ps[7][:, b * 8:(b + 1) * 8], gT[0:8, b * 128:(b + 1) * 128], identf[0:8, 0:8], True, True),
                 reads=[b_gT, self.b_cst], writes=[self.bps[7]])
        S.op("dve", CP(lg[:, :, :], self.ps[7][:, 0:32].rearrange("p (b e) -> p b e", e=8)), reads=[self.bps[7]], writes=[b_lg])

    def moe_router_b(self, ybuf, b_y, gT, b_gT, lg, b_lg, sm, b_sm):
        S = self.S
        identf = self.cstf[:, CF_ID:CF_ID + 128]
        AXX = mybir.AxisListType.X
        for b in range(4):
            L = lg[:, b, :]
            m1 = sm[:, b * 8 + 0:b * 8 + 1]; m2 = sm[:, b * 8 + 1:b * 8 + 2]; nm1 = sm[:, b * 8 + 2:b * 8 + 3]
            den = sm[:, b * 8 + 3:b * 8 + 4]
            t8 = sm[:, 32 + b * 8:32 + b * 8 + 8]
            S.op("dve", lambda e, m1=m1, L=L: e.reduce_max(out=m1, in_=L, axis=AXX), reads=[b_lg], writes=[b_sm])
            S.op("dve", TS(t8, L, m1, -1e30, ALU.is_ge, ALU.mult), reads=[b_lg, b_sm], writes=[b_sm])
            S.op("dve", TT(t8, t8, L, ALU.add), reads=[b_sm, b_lg], writes=[b_sm])
            S.op("dve", lambda e, m2=m2, t8=t8: e.reduce_max(out=m2, in_=t8, axis=AXX), reads=[b_sm], writes=[b_sm])
            S.op("dve", TS(nm1, m1, -1.0, None, ALU.mult), reads=[b_sm], writes=[b_sm])
            S.op("dve", TS(t8, L, m2, None, ALU.is_ge), reads=[b_lg, b_sm], writes=[b_sm])
            S.op("act", ACT(L, L, AF.Exp, bias=nm1), reads=[b_lg, b_sm], writes=[b_lg])
            S.op("dve", TT(L, L, t8, ALU.mult), reads=[b_lg, b_sm], writes=[b_lg])
            S.op("dve", lambda e, den=den, L=L: e.reduce_sum(out=den, in_=L, axis=AXX), reads=[b_lg], writes=[b_sm])
            S.op("dve", RECIP(den, den), reads=[b_sm], writes=[b_sm])
            S.op("dve", TS(L, L, den, None, ALU.mult), reads=[b_lg, b_sm], writes=[b_lg])
        for b in range(4):
            S.op("pe", MM(self.ps[6][0:8, b * 128:(b + 1) * 128], lg[:, b, :], identf, True, True),
                 reads=[b_lg, self.b_cst], writes=[self.bps[6]])
        S.op("act", ACT(gT[0:8, :], self.ps[6][0:8, :], AF.Copy), reads=[self.bps[6]], writes=[b_gT])
        for dc in range(8):
            S.op("dve", TS(ybuf[:, dc, :], ybuf[:, dc, :], ALPHA, None, ALU.mult), reads=[b_y[dc]], writes=[b_y[dc]])

    def moe_experts(self, j, ybuf, b_y, x1b, b_x1b, hT, b_h, w13, b_w13, w2d, b_w2d, sa, b_sa, wcnt, gb, b_gb, gT, b_gT, step=None, moe=True):
        S = self.S
        for e_ in range(8 if moe else 2):
            if moe:
                S.op("pe", MM(self.ps[7][:, :], self.cstf[0:8, CF_SE + e_ * 128: CF_SE + (e_ + 1) * 128], gT[0:8, :], True, True),
                     reads=[b_gT, self.b_cst], writes=[self.bps[7]])
                S.op("act", ACT(gb[:, :], self.ps[7][:, :], AF.Copy), reads=[self.bps[7]], writes=[b_gb])
            s13 = self.sE13[j][e_] if moe else self.sD13[j][e_]
            s2 = self.sE2[j][e_] if moe else self.sD2[j][e_]
            k13 = ("e13", j, e_) if moe else ("d13", j, e_)
            k2 = ("e2", j, e_) if moe else ("d2", j, e_)
            for f in range(11):
                s = wcnt[0] % 2; wcnt[0] += 1
                S.op("sp", DMA(w13[s][0], s13[0][f]), reads=[self.wb[k13 + (0, f)]], writes=[b_w13[s][0]], dma=True)
                S.op("sp", DMA(w13[s][1], s13[1][f]), reads=[self.wb[k13 + (1, f)]], writes=[b_w13[s][1]], dma=True)
                pa, pb = (0, 1) if f % 2 == 0 else (2, 3)
                for q, p in ((0, pa), (1, pb)):
                    for dc in range(8):
                        S.op("pe", MM(self.ps[p][:, :], w13[s][q][:, dc, :], x1b[:, dc, :], dc == 0, dc == 7),
                             reads=[b_w13[s][q], b_x1b], writes=[self.bps[p]])
                i = f % 2
                S.op("act", ACT(sa[i][:, :], self.ps[pa][:, :], AF.Silu), reads=[self.bps[pa]], writes=[b_sa[i]])
                if moe:
                    S.op("dve", TT(sa[i][:, :], sa[i][:, :], self.ps[pb][:, :], ALU.mult), reads=[b_sa[i], self.bps[pb]], writes=[b_sa[i]])
                    S.op("dve", TT(hT[:, f, :], sa[i][:, :], gb[:, :], ALU.mult), reads=[b_sa[i], b_gb], writes=[b_h[f]])
                else:
                    S.op("dve", TT(hT[:, f, :], sa[i][:, :], self.ps[pb][:, :], ALU.mult), reads=[b_sa[i], self.bps[pb]], writes=[b_h[f]])
                if step is not None:
                    step()
            for dc in range(8):
                s = wcnt[1] % 2; wcnt[1] += 1
                S.op("sp", DMA(w2d[s], s2[dc]), reads=[self.wb[k2 + (dc,)]], writes=[b_w2d[s]], dma=True)
                p = dc % 4
                for f in range(11):
                    S.op("pe", MM(self.ps[p][:, :], w2d[s][:, f, :], hT[:, f, :], f == 0, f == 10),
                         reads=[b_w2d[s], b_h[f]], writes=[self.bps[p]])
                S.op("dve", TT(ybuf[:, dc, :], ybuf[:, dc, :], self.ps[p][:, :], ALU.add),
                     reads=[b_y[dc], self.bps[p]], writes=[b_y[dc]])
                if step is not None:
                    step()


def _consts():
    cb = np.zeros((128, CB_N), np.float32)
    cb[:, CB_ID:CB_ID + 128] = np.eye(128, dtype=np.float32)
    k = np.arange(128)[:, None]; q = np.arange(128)[None, :]
    cb[:, CB_NM:CB_NM + 128] = np.where(k > q, -30000.0, 0.0)
    cb[:, CB_ONE:CB_ONE + 128] = 1.0
    bd = np.zeros((128, 128), np.float32)
    bd[:64, :64] = 1.0 / 64; bd[64:, 64:] = 1.0 / 64
    cb[:, CB_BD:CB_BD + 128] = bd
    for h in range(4):
        sq = np.zeros((128, 68), np.float32); sk = np.zeros((128, 68), np.float32)
        sq[h, 64] = 8.0; sq[32 + h, 65] = 8.0; sq[96, 66] = 1.0; sq[96, 67] = 1.0
        sk[96, 64] = 1.0; sk[96, 65] = 1.0; sk[h, 66] = -8.0; sk[32 + h, 67] = -8.0
        cb[:, CB_SQ + h * 68:CB_SQ + (h + 1) * 68] = sq
        cb[:, CB_SK + h * 68:CB_SK + (h + 1) * 68] = sk
    cf = np.zeros((128, CF_N), np.float32)
    cf[:, CF_ONE:CF_ONE + 128] = 1.0
    cf[:, CF_ID:CF_ID + 128] = np.eye(128, dtype=np.float32)
    inv_freq = (10000.0 ** (-np.arange(0, 32, 2, dtype=np.float32) / np.float32(32))).astype(np.float32)
    cf[64:80, CF_IF] = inv_freq; cf[80:96, CF_IF] = inv_freq
    cf[64:80, CF_NS] = -1.0; cf[80:96, CF_NS] = 1.0
    for e in range(8):
        cf[e, CF_SE + e * 128:CF_SE + (e + 1) * 128] = 1.0
    return cb, cf


def _prep(inp):
    f = lambda a: np.ascontiguousarray(np.asarray(a, dtype=np.float32))
    w_in = f(inp["w_in"])
    L = w_in.shape[0]
    z64 = np.zeros((L, D, 64), np.float32)
    kr = w_in[:, :, 384:416]
    krs = np.concatenate([kr[:, :, 16:32], kr[:, :, 0:16]], axis=-1)
    fl = np.zeros((L, D, 128), np.float32)
    fl[:, :, 0:4] = w_in[:, :, 1184:1188]; fl[:, :, 32:36] = w_in[:, :, 1184:1188]
    w_inL = np.concatenate([w_in[:, :, 0:384], z64, kr, z64, krs, fl, w_in[:, :, 1188:1700], w_in[:, :, 416:1184]], axis=-1)
    assert w_inL.shape[-1] == WIN
    w_uq = f(inp["w_uq"])
    parts = []
    for h in range(8):
        parts.append(w_uq[:, :, h * 96:(h + 1) * 96])
    for h in range(8):
        b = h * 96
        parts.append(np.concatenate([np.zeros((L, 256, 64), np.float32), w_uq[:, :, b + 80:b + 96], w_uq[:, :, b + 64:b + 80]], axis=-1))
    w_uqL = np.concatenate(parts, axis=-1)
    pcols = np.zeros((128, DEPTH * NP), np.float32)
    for l in range(DEPTH):
        o = l * NP
        pcols[:, o + PC_QG:o + PC_QG + 2] = f(inp["mla_q_norm_g"])[l].reshape(2, 128).T
        pcols[:, o + PC_KVG] = f(inp["mla_kv_norm_g"])[l]
        fb = f(inp["fox_forget_b"])[l]
        pcols[0:4, o + PC_FB] = fb; pcols[32:36, o + PC_FB] = fb
        cw = f(inp["conv_w"])[l]
        for j in range(31):
            pcols[:, o + PC_CW + j * 2:o + PC_CW + j * 2 + 2] = cw[j].reshape(2, 128).T
        pcols[:, o + PC_CB:o + PC_CB + 2] = f(inp["conv_b"])[l].reshape(2, 128).T
        pcols[:, o + PC_CNG:o + PC_CNG + 2] = f(inp["conv_norm_g"])[l].reshape(2, 128).T
        pcols[:, o + PC_CNB:o + PC_CNB + 2] = f(inp["conv_norm_b"])[l].reshape(2, 128).T
        pcols[:, o + PC_MOG:o + PC_MOG + 4] = f(inp["mla_out_norm_g"])[l].reshape(4, 128).T
        pcols[:, o + PC_FOG:o + PC_FOG + 2] = f(inp["fox_out_norm_g"])[l].reshape(2, 128).T
        pcols[:, o + PC_L1G:o + PC_L1G + 8] = f(inp["ln1_g"])[l].reshape(8, 128).T
        pcols[:, o + PC_L1B:o + PC_L1B + 8] = f(inp["ln1_b"])[l].reshape(8, 128).T
        pcols[:, o + PC_L2G:o + PC_L2G + 8] = f(inp["ln2_g"])[l].reshape(8, 128).T
        pcols[:, o + PC_L2B:o + PC_L2B + 8] = f(inp["ln2_b"])[l].reshape(8, 128).T
    cb, cf = _consts()
    shared = {
        "w_inL": w_inL, "w_uqL": w_uqL, "w_ukv": f(inp["w_ukv"]), "w_out": f(inp["w_out"]),
        "dense_w1": f(inp["dense_w1"]), "dense_w3": f(inp["dense_w3"]), "dense_w2": f(inp["dense_w2"]),
        "router_w": f(inp["router_w"]), "expert_w1": f(inp["expert_w1"]), "expert_w3": f(inp["expert_w3"]),
        "expert_w2": f(inp["expert_w2"]), "pcols": pcols, "cstb": cb, "cstf": cf,
    }
    x = np.asarray(inp["x"], dtype=np.float32)
    pos = np.asarray(inp["positions"]).astype(np.int32)
    maps = []
    for b in range(x.shape[0]):
        m = dict(shared)
        m["xT"] = np.ascontiguousarray(x[b].T).reshape(8, 128, SEQ)
        m["posrep"] = np.ascontiguousarray(np.broadcast_to(pos[b][None, :], (128, SEQ)))
        maps.append(m)
    return maps


_PROG = {}


def run(inputs, n_layers=DEPTH, dbg=None, cores=None, trace=False):
    maps = _prep(inputs)
    key = (n_layers, dbg)
    if key not in _PROG:
        _PROG[key] = Prog(n_layers, dbg)
    prog = _PROG[key]
    if cores is None:
        cores = list(range(len(maps)))
    res = run_bass_kernel_spmd(prog.nc, [maps[i] for i in cores], core_ids=list(range(len(cores))), trace=trace)
    return prog, res


def kernel(**inputs):
    prog, res = run(inputs)
    outs = []
    for r in res.results:
        outs.append(np.asarray(r["outT"]).reshape(D, SEQ).T)
    return np.ascontiguousarray(np.stack(outs, axis=0).astype(np.float32))
```
